# Optimizing a Trainium2 kernel written in Bass

```python
import math
import jax, jax.numpy as jnp
from jax import lax
import numpy as np

D_MODEL = 1024
BATCH = 4
SEQ = 8192
DEPTH = 2

MLA_HEADS = 8
MLA_Q_RANK = 512
MLA_KV_RANK = 256
MLA_NOPE = 128
MLA_ROPE = 64
MLA_V = 128
ROPE_THETA = 10000.0
Q_BLOCK = 128

NSA_HEADS = 16
NSA_GROUPS = 4
NSA_DK = 96
NSA_DV = 64
CMP_LEN = 32
CMP_STRIDE = 16
SEL_BLOCK = 64
SEL_TOPN = 16
WINDOW = 512
NSA_Q_BLOCK = 64
N_BRANCH = 3
FORCE = 1e6

REL_BUCKETS = 32
REL_MAX_DIST = 128

D_FF = 2816
N_EXPERTS = 8
TOP_K = 2
D_FF_EXPERT = 3584
MOE_BLOCK = 256

LN_EPS = 1e-5
RMS_EPS = 1e-6

kernel_name = 'hybrid_mla_nsa_deepnorm_moe'


def layer_norm(x, g, b):
    xf = x.astype(jnp.float32)
    mu = jnp.mean(xf, -1, keepdims=True)
    var = jnp.mean(jnp.square(xf - mu), -1, keepdims=True)
    return ((xf - mu) * lax.rsqrt(var + LN_EPS) * g + b).astype(x.dtype)


def rms_norm(x, g):
    xf = x.astype(jnp.float32)
    return (xf * lax.rsqrt(jnp.mean(xf * xf, -1, keepdims=True) + RMS_EPS) * g).astype(x.dtype)


def rope(x, pos):
    half = x.shape[-1] // 2
    freq = ROPE_THETA ** (-jnp.arange(half, dtype=jnp.float32) / half)
    ang = pos.astype(jnp.float32)[:, None] * freq[None, :]
    shp = (x.shape[1],) + (1,) * (x.ndim - 3) + (half,)
    cos, sin = jnp.cos(ang).reshape(shp), jnp.sin(ang).reshape(shp)
    x1, x2 = x[..., :half], x[..., half:]
    return jnp.concatenate([x1 * cos - x2 * sin, x1 * sin + x2 * cos], -1).astype(x.dtype)


def rel_bucket(dist):
    n = jnp.maximum(dist, 0)
    max_exact = REL_BUCKETS // 2
    nf = jnp.maximum(n, 1).astype(jnp.float32)
    large = max_exact + (jnp.log(nf / max_exact) / math.log(REL_MAX_DIST / max_exact)
                         * (REL_BUCKETS - max_exact)).astype(jnp.int32)
    large = jnp.minimum(large, REL_BUCKETS - 1)
    return jnp.where(n < max_exact, n, large)


def masked_softmax(s, mask):
    s = jnp.where(mask, s.astype(jnp.float32), -jnp.inf)
    m = jnp.max(s, -1, keepdims=True)
    m = jnp.where(jnp.isfinite(m), m, 0.0)
    p = jnp.exp(s - m)
    return p / jnp.maximum(jnp.sum(p, -1, keepdims=True), 1e-30)


def mla_mixer(x, w_in, q_norm, w_q_up, kv_norm, w_kv_up, w_out):
    B, S, _ = x.shape
    H = MLA_HEADS
    pos = jnp.arange(S)
    lat = x @ w_in
    q_lat, kv_lat, k_rope = jnp.split(lat, [MLA_Q_RANK, MLA_Q_RANK + MLA_KV_RANK], axis=-1)
    q = (rms_norm(q_lat, q_norm) @ w_q_up).reshape(B, S, H, MLA_NOPE + MLA_ROPE)
    q_nope, q_rope = q[..., :MLA_NOPE], rope(q[..., MLA_NOPE:], pos)
    kv = (rms_norm(kv_lat, kv_norm) @ w_kv_up).reshape(B, S, H, MLA_NOPE + MLA_V)
    k_nope, v = kv[..., :MLA_NOPE], kv[..., MLA_NOPE:]
    k_rope = rope(k_rope, pos)
    scale = (MLA_NOPE + MLA_ROPE) ** -0.5

    def block(i):
        s0 = i * Q_BLOCK
        qn = lax.dynamic_slice_in_dim(q_nope, s0, Q_BLOCK, 1)
        qr = lax.dynamic_slice_in_dim(q_rope, s0, Q_BLOCK, 1)
        s = (jnp.einsum('bqhd,bkhd->bhqk', qn, k_nope)
             + jnp.einsum('bqhr,bkr->bhqk', qr, k_rope)) * scale
        qpos = s0 + jnp.arange(Q_BLOCK)
        p = masked_softmax(s, qpos[:, None] >= pos[None, :])
        return jnp.einsum('bhqk,bkhd->bqhd', p.astype(v.dtype), v)

    o = lax.map(block, jnp.arange(S // Q_BLOCK))
    o = jnp.moveaxis(o, 0, 1).reshape(B, S, H * MLA_V)
    return o @ w_out


def compress_blocks(blocks, pe, w1, w2):
    h = jnp.einsum('bnlgd,lde->bnge', blocks + pe[:, None, :], w1)
    return jax.nn.gelu(h) @ w2


def nsa_mixer(x, w_in, pe_k, w1_k, w2_k, pe_v, w1_v, w2_v, rel_bias, w_out):
    B, S, _ = x.shape
    H, G, DK, DV = NSA_HEADS, NSA_GROUPS, NSA_DK, NSA_DV
    HG = H // G
    sizes = [H * DK, G * DK, G * DV, G * DK, G * DV, G * DK, G * DV, H * N_BRANCH]
    cuts = [int(c) for c in np.cumsum(sizes)[:-1]]
    q, kc_tok, vc_tok, ks_tok, vs_tok, kw_tok, vw_tok, gate = jnp.split(x @ w_in, cuts, axis=-1)
    q = q.reshape(B, S, G, HG, DK)
    kc_tok, ks_tok, kw_tok = (t.reshape(B, S, G, DK) for t in (kc_tok, ks_tok, kw_tok))
    vc_tok, vs_tok, vw_tok = (t.reshape(B, S, G, DV) for t in (vc_tok, vs_tok, vw_tok))
    gate = jax.nn.sigmoid(gate.astype(jnp.float32)).reshape(B, S, G, HG, N_BRANCH)
    scale = DK ** -0.5
    rb = rel_bias.reshape(REL_BUCKETS, G, HG)

    nc = (S - CMP_LEN) // CMP_STRIDE + 1
    cstart = jnp.arange(nc) * CMP_STRIDE
    cend = cstart + CMP_LEN - 1
    tok = cstart[:, None] + jnp.arange(CMP_LEN)[None, :]
    k_cmp = compress_blocks(kc_tok[:, tok], pe_k, w1_k, w2_k)
    v_cmp = compress_blocks(vc_tok[:, tok], pe_v, w1_v, w2_v)

    nb = S // SEL_BLOCK
    n_sel = min(SEL_TOPN, nb)
    sstart = jnp.arange(nb) * SEL_BLOCK
    overlap = ((cstart[:, None] <= sstart[None, :] + SEL_BLOCK - 1)
               & (cend[:, None] >= sstart[None, :])).astype(jnp.float32)
    k_blk = ks_tok.reshape(B, nb, SEL_BLOCK, G, DK).transpose(0, 3, 1, 2, 4)
    v_blk = vs_tok.reshape(B, nb, SEL_BLOCK, G, DV).transpose(0, 3, 1, 2, 4)
    b_ix = jnp.arange(B)[:, None, None, None]
    g_ix = jnp.arange(G)[None, :, None, None]
    g_ix5 = jnp.arange(G)[None, :, None, None, None]

    kw_pad = jnp.pad(kw_tok, ((0, 0), (WINDOW, 0), (0, 0), (0, 0)))
    vw_pad = jnp.pad(vw_tok, ((0, 0), (WINDOW, 0), (0, 0), (0, 0)))
    QB = NSA_Q_BLOCK
    span = WINDOW + QB

    def head_bias(bucket):
        return rb[bucket].transpose(2, 3, 0, 1)

    def block(i):
        s0 = i * QB
        qpos = s0 + jnp.arange(QB)
        qi = lax.dynamic_slice_in_dim(q, s0, QB, 1)
        gi = lax.dynamic_slice_in_dim(gate, s0, QB, 1)
        dist_c = qpos[:, None] - cend[None, :]
        s_c = jnp.einsum('bqghd,bngd->bghqn', qi, k_cmp) * scale + head_bias(rel_bucket(dist_c))
        p_c = masked_softmax(s_c, dist_c >= 0)
        o_c = jnp.einsum('bghqn,bngd->bqghd', p_c.astype(v_cmp.dtype), v_cmp)
        imp = jnp.einsum('bghqn,nj->bgqj', p_c, overlap)
        cur = (qpos // SEL_BLOCK)[:, None]
        blk = jnp.arange(nb)[None, :]
        forced = (blk == 0) | (blk == cur) | (blk == cur - 1)
        imp = jnp.where(blk > cur, -FORCE, jnp.where(forced, FORCE, imp))
        _, sel = lax.top_k(imp, n_sel)
        k_s = k_blk[b_ix, g_ix, sel]
        v_s = v_blk[b_ix, g_ix, sel]
        dist_s = qpos[:, None, None] - (sel[..., None] * SEL_BLOCK + jnp.arange(SEL_BLOCK))
        bias_s = rb[rel_bucket(dist_s), g_ix5].transpose(0, 1, 5, 2, 3, 4)
        s_s = jnp.einsum('bqghd,bgqnkd->bghqnk', qi, k_s) * scale + bias_s
        m = n_sel * SEL_BLOCK
        p_s = masked_softmax(s_s.reshape(B, G, HG, QB, m), (dist_s >= 0).reshape(B, G, 1, QB, m))
        o_s = jnp.einsum('bghqm,bgqmd->bqghd', p_s.astype(v_s.dtype), v_s.reshape(B, G, QB, m, DV))
        k_w = lax.dynamic_slice_in_dim(kw_pad, s0, span, 1)
        v_w = lax.dynamic_slice_in_dim(vw_pad, s0, span, 1)
        kpos_w = s0 - WINDOW + jnp.arange(span)
        dist_w = qpos[:, None] - kpos_w[None, :]
        mask_w = (dist_w >= 0) & (dist_w < WINDOW) & (kpos_w[None, :] >= 0)
        s_w = jnp.einsum('bqghd,bkgd->bghqk', qi, k_w) * scale + head_bias(rel_bucket(dist_w))
        p_w = masked_softmax(s_w, mask_w)
        o_w = jnp.einsum('bghqk,bkgd->bqghd', p_w.astype(v_w.dtype), v_w)
        o = gi[..., 0:1] * o_c + gi[..., 1:2] * o_s + gi[..., 2:3] * o_w
        return o.astype(x.dtype)

    o = lax.map(block, jnp.arange(S // QB))
    o = jnp.moveaxis(o, 0, 1).reshape(B, S, H * DV)
    return o @ w_out


def swiglu(x, w_gate, w_up, w_down):
    return (jax.nn.silu(x @ w_gate) * (x @ w_up)) @ w_down


def moe_swiglu(x, w_router, w_gate, w_up, w_down):
    B, S, D = x.shape
    T = B * S
    A = T * TOP_K
    xt = x.reshape(T, D)
    logits = (xt @ w_router).astype(jnp.float32)
    top_val, top_idx = lax.top_k(logits, TOP_K)
    wts = jax.nn.softmax(top_val, -1)
    exp_flat = top_idx.reshape(A)
    order = jnp.argsort(exp_flat)
    exp_sorted = exp_flat[order]
    tok_sorted = order // TOP_K
    w_sorted = wts.reshape(A)[order]
    counts = jnp.bincount(exp_flat, length=N_EXPERTS)
    padded = ((counts + MOE_BLOCK - 1) // MOE_BLOCK) * MOE_BLOCK
    grp_start = jnp.cumsum(counts) - counts
    pad_end = jnp.cumsum(padded)
    pad_start = pad_end - padded
    dest = pad_start[exp_sorted] + (jnp.arange(A) - grp_start[exp_sorted])
    n_blocks = (A + MOE_BLOCK - 1) // MOE_BLOCK + N_EXPERTS
    R = n_blocks * MOE_BLOCK
    row_tok = jnp.zeros((R,), jnp.int32).at[dest].set(tok_sorted.astype(jnp.int32))
    row_w = jnp.zeros((R,), jnp.float32).at[dest].set(w_sorted)
    block_expert = jnp.minimum(
        jnp.searchsorted(pad_end, jnp.arange(n_blocks) * MOE_BLOCK, side='right'), N_EXPERTS - 1)

    def expert_block(args):
        toks, wr, e = args
        xb = xt[toks]
        h = jax.nn.silu(xb @ w_gate[e]) * (xb @ w_up[e])
        return (h @ w_down[e]) * wr[:, None].astype(xb.dtype)

    out = lax.map(expert_block, (row_tok.reshape(n_blocks, MOE_BLOCK),
                                 row_w.reshape(n_blocks, MOE_BLOCK), block_expert))
    y = jnp.zeros_like(xt).at[row_tok].add(out.reshape(R, D))
    return y.reshape(B, S, D)


def setup_inputs(seed: int = 0) -> dict:
    key = jax.random.key(seed)
    ks = jax.random.split(key, 28)
    f32 = jnp.float32
    ne, no = (DEPTH + 1) // 2, DEPTH // 2
    beta = (8.0 * DEPTH) ** -0.25
    D = D_MODEL
    nsa_in = (NSA_HEADS * NSA_DK + 3 * NSA_GROUPS * NSA_DK + 3 * NSA_GROUPS * NSA_DV
              + NSA_HEADS * N_BRANCH)

    def w(k, shape, fan_in, s=1.0):
        return jax.random.normal(k, shape, f32) * (s * fan_in ** -0.5)

    def gain(k, shape):
        return 1.0 + 0.02 * jax.random.normal(k, shape, f32)

    def small(k, shape, s=0.02):
        return s * jax.random.normal(k, shape, f32)

    return {
        'x': jax.random.normal(ks[0], (BATCH, SEQ, D), f32),
        'mla_w_in': w(ks[1], (ne, D, MLA_Q_RANK + MLA_KV_RANK + MLA_ROPE), D),
        'mla_q_norm': gain(ks[2], (ne, MLA_Q_RANK)),
        'mla_w_q_up': w(ks[3], (ne, MLA_Q_RANK, MLA_HEADS * (MLA_NOPE + MLA_ROPE)), MLA_Q_RANK),
        'mla_kv_norm': gain(ks[4], (ne, MLA_KV_RANK)),
        'mla_w_kv_up': w(ks[5], (ne, MLA_KV_RANK, MLA_HEADS * (MLA_NOPE + MLA_V)), MLA_KV_RANK),
        'mla_w_out': w(ks[6], (ne, MLA_HEADS * MLA_V, D), MLA_HEADS * MLA_V, beta),
        'nsa_w_in': w(ks[7], (no, D, nsa_in), D),
        'nsa_cmp_pe_k': small(ks[8], (no, CMP_LEN, NSA_DK), 0.1),
        'nsa_cmp_w1_k': w(ks[9], (no, CMP_LEN, NSA_DK, NSA_DK), CMP_LEN * NSA_DK),
        'nsa_cmp_w2_k': w(ks[10], (no, NSA_DK, NSA_DK), NSA_DK),
        'nsa_cmp_pe_v': small(ks[11], (no, CMP_LEN, NSA_DV), 0.1),
        'nsa_cmp_w1_v': w(ks[12], (no, CMP_LEN, NSA_DV, NSA_DV), CMP_LEN * NSA_DV),
        'nsa_cmp_w2_v': w(ks[13], (no, NSA_DV, NSA_DV), NSA_DV),
        'nsa_w_out': w(ks[14], (no, NSA_HEADS * NSA_DV, D), NSA_HEADS * NSA_DV, beta),
        'rel_bias': small(ks[15], (REL_BUCKETS, NSA_HEADS), 0.3),
        'ffn_w_gate': w(ks[16], (ne, D, D_FF), D),
        'ffn_w_up': w(ks[17], (ne, D, D_FF), D),
        'ffn_w_down': w(ks[18], (ne, D_FF, D), D_FF, beta),
        'moe_w_router': w(ks[19], (no, D, N_EXPERTS), D),
        'moe_w_gate': w(ks[20], (no, N_EXPERTS, D, D_FF_EXPERT), D),
        'moe_w_up': w(ks[21], (no, N_EXPERTS, D, D_FF_EXPERT), D),
        'moe_w_down': w(ks[22], (no, N_EXPERTS, D_FF_EXPERT, D), D_FF_EXPERT, beta),
        'ln_mix_g': gain(ks[23], (DEPTH, D)),
        'ln_mix_b': small(ks[24], (DEPTH, D)),
        'ln_ffn_g': gain(ks[25], (DEPTH, D)),
        'ln_ffn_b': small(ks[26], (DEPTH, D)),
    }


def reference(x, mla_w_in, mla_q_norm, mla_w_q_up, mla_kv_norm, mla_w_kv_up, mla_w_out,
              nsa_w_in, nsa_cmp_pe_k, nsa_cmp_w1_k, nsa_cmp_w2_k, nsa_cmp_pe_v, nsa_cmp_w1_v,
              nsa_cmp_w2_v, nsa_w_out, rel_bias, ffn_w_gate, ffn_w_up, ffn_w_down,
              moe_w_router, moe_w_gate, moe_w_up, moe_w_down,
              ln_mix_g, ln_mix_b, ln_ffn_g, ln_ffn_b):
    alpha = (2.0 * DEPTH) ** 0.25
    for i in range(DEPTH):
        j = i // 2
        if i % 2 == 0:
            h = mla_mixer(x, mla_w_in[j], mla_q_norm[j], mla_w_q_up[j], mla_kv_norm[j],
                          mla_w_kv_up[j], mla_w_out[j])
        else:
            h = nsa_mixer(x, nsa_w_in[j], nsa_cmp_pe_k[j], nsa_cmp_w1_k[j], nsa_cmp_w2_k[j],
                          nsa_cmp_pe_v[j], nsa_cmp_w1_v[j], nsa_cmp_w2_v[j], rel_bias, nsa_w_out[j])
        x = layer_norm(alpha * x + h, ln_mix_g[i], ln_mix_b[i])
        if i % 2 == 0:
            f = swiglu(x, ffn_w_gate[j], ffn_w_up[j], ffn_w_down[j])
        else:
            f = moe_swiglu(x, moe_w_router[j], moe_w_gate[j], moe_w_up[j], moe_w_down[j])
        x = layer_norm(alpha * x + f, ln_ffn_g[i], ln_ffn_b[i])
    return x
```

```python
import numpy as np
import concourse.bass as bass
import concourse.mybir as mybir
from concourse.bass_utils import run_bass_kernel_spmd
from contextlib import ExitStack
import ml_dtypes

F32 = mybir.dt.float32
BF16 = mybir.dt.bfloat16
I32 = mybir.dt.int32
U32 = mybir.dt.uint32
ALU = mybir.AluOpType
AF = mybir.ActivationFunctionType
AX = mybir.AxisListType
NPBF = ml_dtypes.bfloat16


class Tok:
    __slots__ = ("w", "r")

    def __init__(self):
        self.w = None
        self.r = {}


class KB:
    def __init__(self, nc, es, ndma=24):
        self.nc = nc
        self.es = es
        self.eng = {"pe": nc.tensor, "act": nc.scalar, "dve": nc.vector,
                    "pool": nc.gpsimd, "sp": nc.sync}
        self.sems = {}
        self.cnt = {}
        for e in self.eng:
            self.sems[e] = es.enter_context(nc.semaphore("s_" + e))
            self.cnt[e] = 0
        self.ndma = ndma
        for i in range(ndma):
            k = ("d", i)
            self.sems[k] = es.enter_context(nc.semaphore("d%d" % i))
            self.cnt[k] = 0
        self.rr = 0
        self.seen = {e: {} for e in self.eng}
        self.ntens = 0

    def sb(self, shape, dt, name=None):
        self.ntens += 1
        return self.es.enter_context(
            self.nc.sbuf_tensor(name or ("t%d" % self.ntens), list(shape), dt))

    def ps(self, shape, dt=F32, name=None):
        self.ntens += 1
        return self.es.enter_context(
            self.nc.psum_tensor(name or ("p%d" % self.ntens), list(shape), dt))

    def _wait(self, e, deps):
        need = {}
        for d in deps:
            if d is None:
                continue
            k, v = d
            if v > need.get(k, 0):
                need[k] = v
        for k, v in need.items():
            if self.seen[e].get(k, 0) >= v:
                continue
            if k == e and e == "pe":
                continue
            self.eng[e].wait_ge(self.sems[k], v)
            self.seen[e][k] = v

    def _deps(self, reads, writes):
        deps = []
        for t in reads:
            deps.append(t.w)
        for t in writes:
            deps.append(t.w)
            deps.extend(t.r.items())
        return deps

    def _mark(self, me, reads, writes):
        k, v = me
        for t in reads:
            if t.r.get(k, 0) < v:
                t.r[k] = v
        for t in writes:
            t.w = me
            t.r = {}

    def op(self, e, fn, reads=(), writes=()):
        self._wait(e, self._deps(reads, writes))
        inst = fn(self.eng[e])
        self.cnt[e] += 1
        inst.then_inc(self.sems[e], 1)
        self._mark((e, self.cnt[e]), reads, writes)
        return inst

    def dma(self, q, out, in_, reads=(), writes=(), **kw):
        k = ("d", self.rr)
        self.rr = (self.rr + 1) % self.ndma
        deps = self._deps(reads, writes)
        deps.append((k, self.cnt[k]))
        self._wait(q, deps)
        inst = self.eng[q].dma_start(out=out, in_=in_, **kw)
        self.cnt[k] += 16
        inst.then_inc(self.sems[k], 16)
        self._mark((k, self.cnt[k]), reads, writes)
        return inst

    def finish(self):
        e = "sp"
        deps = [(k, v) for k, v in self.cnt.items() if v > 0]
        self._wait(e, deps)


class Ring:
    def __init__(self, bufs):
        self.bufs = bufs
        self.toks = [Tok() for _ in bufs]
        self.i = 0

    def next(self):
        b, t = self.bufs[self.i], self.toks[self.i]
        self.i = (self.i + 1) % len(self.bufs)
        return b, t


def sb_ring(k, n, shape, dt):
    return Ring([k.sb(shape, dt) for _ in range(n)])


def ps_ring(k, n, shape=(128, 512), dt=F32):
    return Ring([k.ps(shape, dt) for _ in range(n)])


def mm_acc(k, pt, ptok, pairs, reads):
    n = len(pairs)
    for i, (l, r) in enumerate(pairs):
        k.op("pe", lambda e, l=l, r=r, i=i: e.matmul(pt, lhsT=l, rhs=r, start=(i == 0), stop=(i == n - 1)),
             reads=reads, writes=[ptok])


def load_w(k, w_dram, kc, dout, q="pool", name=None):
    t = k.sb([128, kc, dout], BF16, name)
    tok = Tok()
    wv = w_dram.rearrange("(c p) o -> p c o", p=128)
    for c in range(kc):
        for o0 in range(0, dout, 2048):
            o1 = min(dout, o0 + 2048)
            k.dma(q, t[:, c, o0:o1], wv[:, c, o0:o1], writes=[tok])
    return t, tok


RMS_EPS = 1e-6


def evac(k, eng, out_ap, in_ap, reads, writes):
    if eng == "act":
        return k.op("act", lambda e: e.activation(out=out_ap, in_=in_ap, func=AF.Copy), reads=reads, writes=writes)
    return k.op(eng, lambda e: e.tensor_copy(out=out_ap, in_=in_ap), reads=reads, writes=writes)


def rms_fm(k, psring, pairs_fn, nch, dim, xin_reads, ones, tones, gt, tg, lat, tlat, sq, tsq, rstd, trstd, outn, toutn):
    for c in range(nch):
        ps, tps = psring.next()
        mm_acc(k, ps[:], tps, pairs_fn(c), xin_reads)
        k.op("act", lambda e: e.activation(out=lat[:, c, :], in_=ps[:], func=AF.Copy), reads=[tps], writes=[tlat])
        k.op("act", lambda e: e.activation(out=sq[:, c, :], in_=ps[:], func=AF.Square), reads=[tps], writes=[tsq])
    ps, tps = psring.next()
    mm_acc(k, ps[:], tps, [(ones[:], sq[:, c, :]) for c in range(nch)], [tones, tsq])
    k.op("act", lambda e: e.activation(out=rstd[:], in_=ps[:], func=AF.Sqrt, scale=1.0 / dim, bias=RMS_EPS),
         reads=[tps], writes=[trstd])
    k.op("dve", lambda e: e.reciprocal(out=rstd[:], in_=rstd[:]), reads=[trstd], writes=[trstd])
    for c in range(nch):
        k.op("dve", lambda e: e.scalar_tensor_tensor(out=outn[:, c, :], in0=lat[:, c, :], scalar=gt[:, c:c + 1],
                                                     in1=rstd[:], op0=ALU.mult, op1=ALU.mult),
             reads=[tlat, tg, trstd], writes=[toutn])


def build_A(NT=4096):
    nc = bass.Bass("TRN2", target_bir_lowering=False)
    dt = lambda n, s, d, kind: nc.dram_tensor(n, s, d, kind=kind).ap()
    xT = dt("xT", [1024, NT], F32, "ExternalInput")
    w_in = dt("w_in", [1024, 896], F32, "ExternalInput")
    w_q = dt("w_q", [512, 2048], F32, "ExternalInput")
    w_kv = dt("w_kv", [256, 2048], F32, "ExternalInput")
    gq = dt("gq", [128, 4], F32, "ExternalInput")
    gkv = dt("gkv", [128, 2], F32, "ExternalInput")
    cosT = dt("cosT", [128, NT], F32, "ExternalInput")
    sinS = dt("sinS", [128, NT], F32, "ExternalInput")
    QnT = dt("QnT", [1024, NT], BF16, "ExternalOutput")
    QrT = dt("QrT", [512, NT], BF16, "ExternalOutput")
    KnT = dt("KnT", [1024, NT], BF16, "ExternalOutput")
    KrT = dt("KrT", [64, NT], BF16, "ExternalOutput")
    V = dt("V", [NT, 1024], BF16, "ExternalOutput")
    with ExitStack() as es:
        k = KB(nc, es)
        win, twin = load_w(k, w_in, 8, 896)
        wq, twq = load_w(k, w_q, 4, 2048)
        wkv, twkv = load_w(k, w_kv, 2, 2048)
        gq_t = k.sb([128, 4], F32); tgq = Tok()
        gkv_t = k.sb([128, 2], F32); tgkv = Tok()
        k.dma("sp", gq_t[:], gq, writes=[tgq])
        k.dma("sp", gkv_t[:], gkv, writes=[tgkv])
        ones = k.sb([128, 128], BF16); tones = Tok()
        k.op("dve", lambda e: e.memset(ones[:], 1.0), writes=[tones])
        xring = sb_ring(k, 2, [128, 8, 512], BF16)
        cring = sb_ring(k, 2, [128, 512], F32)
        sring = sb_ring(k, 2, [128, 512], F32)
        psring = ps_ring(k, 6)
        qlat = k.sb([128, 4, 512], F32); tqlat = Tok()
        sq = k.sb([128, 4, 512], BF16); tsq = Tok()
        rstd = k.sb([128, 512], F32); trstd = Tok()
        qn = k.sb([128, 4, 512], BF16); tqn = Tok()
        kvlat = k.sb([128, 2, 512], F32); tkvlat = Tok()
        sq2 = k.sb([128, 2, 512], BF16); tsq2 = Tok()
        rstd2 = k.sb([128, 512], F32); trstd2 = Tok()
        kvn = k.sb([128, 2, 512], BF16); tkvn = Tok()
        t1r = sb_ring(k, 2, [128, 512], F32)
        t2r = sb_ring(k, 2, [128, 512], F32)
        oring = sb_ring(k, 4, [128, 512], BF16)
        vring = sb_ring(k, 2, [128, 1024], BF16)
        xTv = xT.rearrange("(c p) t -> p c t", p=128)
        ev = ["act", "dve"]
        nev = 0
        for nb in range(NT // 512):
            t0 = nb * 512
            ts_ = slice(t0, t0 + 512)
            xb, txb = xring.next()
            for c in range(8):
                k.dma("pool", xb[:, c, :], xTv[:, c, ts_], writes=[txb])
            cs, tcs = cring.next()
            sn, tsn = sring.next()
            k.dma("sp", cs[:], cosT[:, ts_], writes=[tcs])
            k.dma("sp", sn[:], sinS[:, ts_], writes=[tsn])
            rms_fm(k, psring, lambda c: [(win[:, kc, c * 128:(c + 1) * 128], xb[:, kc, :]) for kc in range(8)],
                   4, 512, [twin, txb], ones, tones, gq_t, tgq, qlat, tqlat, sq, tsq, rstd, trstd, qn, tqn)
            rms_fm(k, psring, lambda c: [(win[:, kc, 512 + c * 128:512 + (c + 1) * 128], xb[:, kc, :]) for kc in range(8)],
                   2, 256, [twin, txb], ones, tones, gkv_t, tgkv, kvlat, tkvlat, sq2, tsq2, rstd2, trstd2, kvn, tkvn)

            def rope_pair(pa, pb, np_, reads, out_dram):
                ps1, tp1 = psring.next()
                mm_acc(k, ps1[:np_, :], tp1, pa, reads)
                ps2, tp2 = psring.next()
                mm_acc(k, ps2[:np_, :], tp2, pb, reads)
                t1, tt1 = t1r.next()
                t2, tt2 = t2r.next()
                k.op("dve", lambda e: e.tensor_tensor(out=t1[:np_, :], in0=ps1[:np_, :], in1=cs[:np_, :], op=ALU.mult),
                     reads=[tp1, tcs], writes=[tt1])
                k.op("dve", lambda e: e.tensor_tensor(out=t2[:np_, :], in0=ps2[:np_, :], in1=sn[:np_, :], op=ALU.mult),
                     reads=[tp2, tsn], writes=[tt2])
                ob, tob = oring.next()
                k.op("pool", lambda e: e.tensor_tensor(out=ob[:np_, :], in0=t1[:np_, :], in1=t2[:np_, :], op=ALU.add),
                     reads=[tt1, tt2], writes=[tob])
                k.dma("sp", out_dram, ob[:np_, :], reads=[tob])

            rope_pair([(win[:, kc, 768:832], xb[:, kc, :]) for kc in range(8)],
                      [(win[:, kc, 832:896], xb[:, kc, :]) for kc in range(8)], 64, [twin, txb], KrT[:, ts_])
            for h in range(8):
                ps, tps = psring.next()
                mm_acc(k, ps[:], tps, [(wq[:, c, h * 128:(h + 1) * 128], qn[:, c, :]) for c in range(4)], [twq, tqn])
                ob, tob = oring.next()
                evac(k, ev[nev % 2], ob[:], ps[:], [tps], [tob]); nev += 1
                k.dma("sp", QnT[h * 128:(h + 1) * 128, ts_], ob[:], reads=[tob])
            for j in range(4):
                rope_pair([(wq[:, c, 1024 + j * 128:1024 + (j + 1) * 128], qn[:, c, :]) for c in range(4)],
                          [(wq[:, c, 1536 + j * 128:1536 + (j + 1) * 128], qn[:, c, :]) for c in range(4)],
                          128, [twq, tqn], QrT[j * 128:(j + 1) * 128, ts_])
            for h in range(8):
                ps, tps = psring.next()
                mm_acc(k, ps[:], tps, [(wkv[:, c, h * 128:(h + 1) * 128], kvn[:, c, :]) for c in range(2)], [twkv, tkvn])
                ob, tob = oring.next()
                evac(k, ev[nev % 2], ob[:], ps[:], [tps], [tob]); nev += 1
                k.dma("sp", KnT[h * 128:(h + 1) * 128, ts_], ob[:], reads=[tob])
            for ts in range(4):
                vb, tvb = vring.next()
                for cc in range(2):
                    ps, tps = psring.next()
                    mm_acc(k, ps[:], tps, [(kvn[:, c, ts * 128:(ts + 1) * 128],
                                            wkv[:, c, 1024 + cc * 512:1024 + (cc + 1) * 512]) for c in range(2)],
                           [twkv, tkvn])
                    evac(k, ev[nev % 2], vb[:, cc * 512:(cc + 1) * 512], ps[:], [tps], [tvb]); nev += 1
                k.dma("sp", V[t0 + ts * 128:t0 + (ts + 1) * 128, :], vb[:], reads=[tvb])
        k.finish()
    return nc


def rope_tables(pos):
    half = 32
    freq = (10000.0 ** (-np.arange(half, dtype=np.float32) / half)).astype(np.float32)
    ang = pos.astype(np.float32)[None, :] * freq[:, None]
    cos, sin = np.cos(ang).astype(np.float32), np.sin(ang).astype(np.float32)
    cosT = np.concatenate([cos, cos, cos, cos], 0)
    sinS = np.concatenate([-sin, sin, -sin, sin], 0)
    return np.ascontiguousarray(cosT), np.ascontiguousarray(sinS)


def prep_A(I):
    w_in = I["mla_w_in"][0]
    kr = w_in[:, 768:832]
    w_in_all = np.ascontiguousarray(np.concatenate([w_in, kr[:, 32:64], kr[:, 0:32]], 1))
    wq = I["mla_w_q_up"][0].reshape(512, 8, 192)
    wqn = wq[:, :, :128].reshape(512, 1024)
    wqr = wq[:, :, 128:]
    wqrp = np.concatenate([wqr[:, :, 32:], wqr[:, :, :32]], 2)
    w_q_all = np.ascontiguousarray(np.concatenate([wqn, wqr.reshape(512, 512), wqrp.reshape(512, 512)], 1))
    wkv = I["mla_w_kv_up"][0].reshape(256, 8, 256)
    w_kv_all = np.ascontiguousarray(np.concatenate([wkv[:, :, :128].reshape(256, 1024), wkv[:, :, 128:].reshape(256, 1024)], 1))
    gq = np.ascontiguousarray(I["mla_q_norm"][0].reshape(4, 128).T)
    gkv = np.ascontiguousarray(I["mla_kv_norm"][0].reshape(2, 128).T)
    return dict(w_in=w_in_all, w_q=w_q_all, w_kv=w_kv_all, gq=gq, gkv=gkv)


MLA_SCALE = 192 ** -0.5


def maxnorm2(k, psring, nblk, a128, ta, a64, ta64, ones, tones, sqr, mx, tmx, res, tres):
    for i in range(nblk):
        sl = slice(i * 512, (i + 1) * 512)
        s1, ts1 = sqr.next()
        k.op("dve", lambda e: e.tensor_tensor(out=s1[:], in0=a128[:, sl], in1=a128[:, sl], op=ALU.mult),
             reads=[ta], writes=[ts1])
        s2, ts2 = sqr.next()
        k.op("pool", lambda e: e.tensor_tensor(out=s2[0:64, :], in0=a64[0:64, sl], in1=a64[0:64, sl], op=ALU.mult),
             reads=[ta64], writes=[ts2])
        ps, tps = psring.next()
        mm_acc(k, ps[:], tps, [(ones[:], s1[:]), (ones[0:64, :], s2[0:64, :])], [tones, ts1, ts2])
        k.op("dve", lambda e: e.reduce_max(out=mx[:, i:i + 1], in_=ps[:], axis=AX.X), reads=[tps], writes=[tmx])
    k.op("dve", lambda e: e.reduce_max(out=res[:], in_=mx[:, 0:nblk], axis=AX.X), reads=[tmx], writes=[tres])


def build_B(S=8192, NH=4):
    nc = bass.Bass("TRN2", target_bir_lowering=False)
    dt = lambda n, s, d, kind: nc.dram_tensor(n, s, d, kind=kind).ap()
    QnT = dt("QnT", [NH, 128, S], BF16, "ExternalInput")
    QrT = dt("QrT", [NH, 64, S], BF16, "ExternalInput")
    KnT = dt("KnT", [NH, 128, S], BF16, "ExternalInput")
    KrT = dt("KrT", [64, S], BF16, "ExternalInput")
    V = dt("V", [NH, S, 128], BF16, "ExternalInput")
    tri = dt("tri", [128, 128], BF16, "ExternalInput")
    O = dt("O", [S, NH * 128], BF16, "ExternalOutput")
    NKT = S // 128
    NQT = S // 512
    with ExitStack() as es:
        k = KB(nc, es)
        ones = k.sb([128, 128], BF16); tones = Tok()
        k.op("dve", lambda e: e.memset(ones[:], 1.0), writes=[tones])
        tri_t = k.sb([128, 128], BF16); ttri = Tok()
        k.dma("sp", tri_t[:], tri, writes=[ttri])
        kr = k.sb([64, S], BF16); tkr = Tok()
        k.dma("sp", kr[:], KrT, writes=[tkr])
        kn = k.sb([128, S], BF16); tkn = Tok()
        qn = k.sb([128, S], BF16); tqn = Tok()
        qr = k.sb([64, S], BF16); tqr = Tok()
        v1 = k.sb([128, NKT, 129], BF16); tv1 = Tok()
        k.op("pool", lambda e: e.memset(v1[:, :, 128:129], 1.0), writes=[tv1])
        sqr = sb_ring(k, 4, [128, 512], BF16)
        mx = k.sb([128, 16], F32); tmx = Tok()
        qm = k.sb([128, 1], F32); tqm = Tok()
        km = k.sb([128, 1], F32); tkm = Tok()
        negM = k.sb([128, 1], F32); tnegM = Tok()
        sring = ps_ring(k, 3)
        accs = [k.ps([128, 512]) for _ in range(4)]
        taccs = [Tok() for _ in range(4)]
        pring = sb_ring(k, 3, [128, 512], BF16)
        oring = sb_ring(k, 2, [128, 4, 128], BF16)
        rl = k.sb([128, 4], F32); trl = Tok()
        for h in range(NH):
            k.dma("sp", kn[:], KnT[h], writes=[tkn])
            k.dma("sp", qn[:], QnT[h], writes=[tqn])
            k.dma("sp", qr[:], QrT[h], writes=[tqr])
            k.dma("sp", v1[:, :, 0:128], V[h].rearrange("(t p) d -> p t d", p=128), writes=[tv1])
            maxnorm2(k, sring, S // 512, qn, tqn, qr, tqr, ones, tones, sqr, mx, tmx, qm, tqm)
            maxnorm2(k, sring, S // 512, kn, tkn, kr, tkr, ones, tones, sqr, mx, tmx, km, tkm)
            k.op("dve", lambda e: e.tensor_tensor(out=negM[:], in0=qm[:], in1=km[:], op=ALU.mult),
                 reads=[tqm, tkm], writes=[tnegM])
            k.op("act", lambda e: e.activation(out=negM[:], in_=negM[:], func=AF.Sqrt), reads=[tnegM], writes=[tnegM])
            k.op("dve", lambda e: e.tensor_scalar(out=negM[:], in0=negM[:], scalar1=-MLA_SCALE * 1.05, scalar2=None,
                                                  op0=ALU.mult), reads=[tnegM], writes=[tnegM])
            tiles = [(qt, kt) for qt in range(NQT) for kt in range(4 * qt + 4)]
            pend = None
            for idx in range(len(tiles) + 1):
                cur = None
                if idx < len(tiles):
                    qt, kt = tiles[idx]
                    j = kt - 4 * qt
                    c0 = max(j, 0) * 128
                    sps, tsp = sring.next()
                    qsl = slice(qt * 512 + c0, (qt + 1) * 512)
                    ksl = slice(kt * 128, (kt + 1) * 128)
                    mm_acc(k, sps[:, c0:512], tsp, [(kn[:, ksl], qn[:, qsl]), (kr[0:64, ksl], qr[0:64, qsl])],
                           [tkn, tqn, tkr, tqr])
                    pt, tpt = pring.next()
                    k.op("act", lambda e: e.activation(out=pt[:, c0:512], in_=sps[:, c0:512], func=AF.Exp,
                                                       scale=MLA_SCALE, bias=negM[:]),
                         reads=[tsp, tnegM], writes=[tpt])
                    if j >= 0:
                        k.op("pool", lambda e: e.tensor_tensor(out=pt[:, c0:c0 + 128], in0=pt[:, c0:c0 + 128],
                                                               in1=tri_t[:], op=ALU.mult),
                             reads=[tpt, ttri], writes=[tpt])
                    cur = (qt, kt, j, pt, tpt)
                if pend is not None:
                    qt_, kt_, j_, pt_, tpt_ = pend
                    for qs in range(max(j_, 0), 4):
                        last = 4 * qt_ + qs
                        k.op("pe", lambda e, qs=qs: e.matmul(accs[qs][:, 0:129], lhsT=pt_[:, qs * 128:(qs + 1) * 128],
                                                             rhs=v1[:, kt_, :], start=(kt_ == 0), stop=(kt_ == last)),
                             reads=[tpt_, tv1], writes=[taccs[qs]])
                    if kt_ == 4 * qt_ + 3:
                        ob, tob = oring.next()
                        for qs in range(4):
                            k.op("dve", lambda e, qs=qs: e.reciprocal(out=rl[:, qs:qs + 1], in_=accs[qs][:, 128:129]),
                                 reads=[taccs[qs]], writes=[trl])
                            k.op("dve", lambda e, qs=qs: e.tensor_scalar(out=ob[:, qs, :], in0=accs[qs][:, 0:128],
                                                                         scalar1=rl[:, qs:qs + 1], scalar2=None,
                                                                         op0=ALU.mult),
                                 reads=[taccs[qs], trl], writes=[tob])
                        k.dma("sp", O[qt_ * 512:(qt_ + 1) * 512, h * 128:(h + 1) * 128].rearrange("(s p) d -> p s d", p=128),
                              ob[:], reads=[tob])
                pend = cur
        k.finish()
    return nc


def tri_mask():
    return np.triu(np.ones((128, 128), np.float32)).astype(NPBF)


ALPHA = 4.0 ** 0.25
LN_EPS = 1e-5


def ln_fm(k, psring, sqring, ones, tones, y, ty, nb, gt, bt, tgb, mean, tmean, rstd, trstd, tmp, ttmp, outs):
    ps1, tp1 = psring.next()
    ps2, tp2 = psring.next()
    for m in range(8):
        yb, tyb = sqring.next()
        k.op("pool", lambda e: e.tensor_copy(out=yb[:, :nb], in_=y[:, m, :]), reads=[ty], writes=[tyb])
        k.op("pe", lambda e: e.matmul(ps1[:, :nb], lhsT=ones[:], rhs=yb[:, :nb], start=(m == 0), stop=(m == 7)),
             reads=[tones, tyb], writes=[tp1])
        ys, tys = sqring.next()
        k.op("act", lambda e: e.activation(out=ys[:, :nb], in_=y[:, m, :], func=AF.Square), reads=[ty], writes=[tys])
        k.op("pe", lambda e: e.matmul(ps2[:, :nb], lhsT=ones[:], rhs=ys[:, :nb], start=(m == 0), stop=(m == 7)),
             reads=[tones, tys], writes=[tp2])
    k.op("act", lambda e: e.activation(out=mean[:, :nb], in_=ps1[:, :nb], func=AF.Copy, scale=1.0 / 1024),
         reads=[tp1], writes=[tmean])
    k.op("dve", lambda e: e.tensor_tensor(out=tmp[:, :nb], in0=mean[:, :nb], in1=mean[:, :nb], op=ALU.mult),
         reads=[tmean], writes=[ttmp])
    k.op("dve", lambda e: e.scalar_tensor_tensor(out=rstd[:, :nb], in0=ps2[:, :nb], scalar=1.0 / 1024, in1=tmp[:, :nb],
                                                 op0=ALU.mult, op1=ALU.subtract), reads=[tp2, ttmp], writes=[trstd])
    k.op("act", lambda e: e.activation(out=rstd[:, :nb], in_=rstd[:, :nb], func=AF.Sqrt, bias=LN_EPS),
         reads=[trstd], writes=[trstd])
    k.op("dve", lambda e: e.reciprocal(out=rstd[:, :nb], in_=rstd[:, :nb]), reads=[trstd], writes=[trstd])
    for m in range(8):
        k.op("dve", lambda e: e.tensor_tensor(out=y[:, m, :], in0=y[:, m, :], in1=mean[:, :nb], op=ALU.subtract),
             reads=[ty, tmean], writes=[ty])
        k.op("dve", lambda e: e.tensor_tensor(out=y[:, m, :], in0=y[:, m, :], in1=rstd[:, :nb], op=ALU.mult),
             reads=[ty, trstd], writes=[ty])
        k.op("act", lambda e: e.activation(out=y[:, m, :], in_=y[:, m, :], func=AF.Identity,
                                           scale=gt[:, m:m + 1], bias=bt[:, m:m + 1]), reads=[ty, tgb], writes=[ty])
        for (o, to) in outs:
            k.op("pool", lambda e: e.tensor_copy(out=o[:, m, :], in_=y[:, m, :]), reads=[ty], writes=[to])


def build_C(NT=4096, NB=256):
    nc = bass.Bass("TRN2", target_bir_lowering=False)
    dt = lambda n, s, d, kind: nc.dram_tensor(n, s, d, kind=kind).ap()
    OT = dt("OT", [1024, NT], BF16, "ExternalInput")
    xT = dt("xT", [1024, NT], F32, "ExternalInput")
    w_out = dt("w_out", [1024, 1024], F32, "ExternalInput")
    w_g = dt("w_g", [1024, 2816], F32, "ExternalInput")
    w_u = dt("w_u", [1024, 2816], F32, "ExternalInput")
    w_d = dt("w_d", [2816, 1024], F32, "ExternalInput")
    lnp = dt("lnp", [128, 4, 8], F32, "ExternalInput")
    x2T = dt("x2T", [1024, NT], F32, "ExternalOutput")
    NF = 22
    with ExitStack() as es:
        k = KB(nc, es)
        wo, two = load_w(k, w_out, 8, 1024)
        wg, twg = load_w(k, w_g, 8, 2816)
        wu, twu = load_w(k, w_u, 8, 2816)
        wd, twd = load_w(k, w_d, NF, 1024)
        ln_t = k.sb([128, 4, 8], F32); tln = Tok()
        k.dma("sp", ln_t[:], lnp, writes=[tln])
        ones = k.sb([128, 128], BF16); tones = Tok()
        k.op("dve", lambda e: e.memset(ones[:], 1.0), writes=[tones])
        psring = ps_ring(k, 7)
        sqring = sb_ring(k, 4, [128, NB], BF16)
        oring = sb_ring(k, 2, [128, 8, NB], BF16)
        yring = sb_ring(k, 2, [128, 8, NB], F32)
        x1b = k.sb([128, 8, NB], BF16); tx1b = Tok()
        hb = k.sb([128, NF, NB], BF16); thb = Tok()
        sgring = sb_ring(k, 3, [128, NB], BF16)
        mean = k.sb([128, NB], F32); tmean = Tok()
        rstd = k.sb([128, NB], F32); trstd = Tok()
        tmp = k.sb([128, NB], F32); ttmp = Tok()
        OTv = OT.rearrange("(c p) t -> p c t", p=128)
        xTv = xT.rearrange("(c p) t -> p c t", p=128)
        x2v = x2T.rearrange("(c p) t -> p c t", p=128)
        for nb in range(NT // NB):
            ts_ = slice(nb * NB, (nb + 1) * NB)
            ob, tob = oring.next()
            k.dma("sp", ob[:], OTv[:, :, ts_], writes=[tob])
            y, ty = yring.next()
            k.dma("sp", y[:], xTv[:, :, ts_], writes=[ty])
            for m in range(8):
                ps, tps = psring.next()
                mm_acc(k, ps[:, :NB], tps, [(wo[:, kc, m * 128:(m + 1) * 128], ob[:, kc, :]) for kc in range(8)], [two, tob])
                k.op("dve", lambda e: e.scalar_tensor_tensor(out=y[:, m, :], in0=y[:, m, :], scalar=ALPHA, in1=ps[:, :NB],
                                                             op0=ALU.mult, op1=ALU.add), reads=[ty, tps], writes=[ty])
            ln_fm(k, psring, sqring, ones, tones, y, ty, NB, ln_t[:, 0, :], ln_t[:, 1, :], tln, mean, tmean, rstd, trstd,
                  tmp, ttmp, [(x1b, tx1b)])
            for f in range(NF):
                psg, tpg = psring.next()
                mm_acc(k, psg[:, :NB], tpg, [(wg[:, kc, f * 128:(f + 1) * 128], x1b[:, kc, :]) for kc in range(8)], [twg, tx1b])
                psu, tpu = psring.next()
                mm_acc(k, psu[:, :NB], tpu, [(wu[:, kc, f * 128:(f + 1) * 128], x1b[:, kc, :]) for kc in range(8)], [twu, tx1b])
                sg, tsg = sgring.next()
                k.op("act", lambda e: e.activation(out=sg[:], in_=psg[:, :NB], func=AF.Silu), reads=[tpg], writes=[tsg])
                k.op("dve", lambda e: e.tensor_tensor(out=hb[:, f, :], in0=sg[:], in1=psu[:, :NB], op=ALU.mult),
                     reads=[tsg, tpu], writes=[thb])
            for m in range(8):
                ps, tps = psring.next()
                mm_acc(k, ps[:, :NB], tps, [(wd[:, f, m * 128:(m + 1) * 128], hb[:, f, :]) for f in range(NF)], [twd, thb])
                k.op("dve", lambda e: e.scalar_tensor_tensor(out=y[:, m, :], in0=y[:, m, :], scalar=ALPHA, in1=ps[:, :NB],
                                                             op0=ALU.mult, op1=ALU.add), reads=[ty, tps], writes=[ty])
            ln_fm(k, psring, sqring, ones, tones, y, ty, NB, ln_t[:, 2, :], ln_t[:, 3, :], tln, mean, tmean, rstd, trstd,
                  tmp, ttmp, [])
            k.dma("sp", x2v[:, :, ts_], y[:], reads=[ty])
        k.finish()
    return nc


def prep_C(I):
    lnp = np.stack([I["ln_mix_g"][0], I["ln_mix_b"][0], I["ln_ffn_g"][0], I["ln_ffn_b"][0]], 0)
    lnp = np.ascontiguousarray(lnp.reshape(4, 8, 128).transpose(2, 0, 1))
    return dict(w_out=I["mla_w_out"][0], w_g=I["ffn_w_gate"][0], w_u=I["ffn_w_up"][0], w_d=I["ffn_w_down"][0], lnp=lnp)


def build_D(NT=4096):
    nc = bass.Bass("TRN2", target_bir_lowering=False)
    dt = lambda n, s, d, kind: nc.dram_tensor(n, s, d, kind=kind).ap()
    xT = dt("xT", [1024, NT], F32, "ExternalInput")
    w_fm = dt("w_fm", [1024, 2944], F32, "ExternalInput")
    w_tm = dt("w_tm", [1024, 560], F32, "ExternalInput")
    QT = dt("QT", [16, 96, NT], BF16, "ExternalOutput")
    KT = dt("KT", [12, 96, NT], BF16, "ExternalOutput")
    VcT = dt("VcT", [4, 64, NT], BF16, "ExternalOutput")
    Vtm = dt("Vtm", [NT, 512], BF16, "ExternalOutput")
    gate = dt("gate", [NT, 48], F32, "ExternalOutput")
    with ExitStack() as es:
        k = KB(nc, es)
        wf, twf = load_w(k, w_fm, 8, 2944)
        wt, twt = load_w(k, w_tm, 8, 560)
        xring = sb_ring(k, 2, [128, 8, 512], BF16)
        psring = ps_ring(k, 6)
        oring = sb_ring(k, 4, [128, 512], BF16)
        gring = sb_ring(k, 2, [128, 48], F32)
        xTv = xT.rearrange("(c p) t -> p c t", p=128)
        ev = ["act", "dve"]
        nev = 0
        for nb in range(NT // 512):
            ts_ = slice(nb * 512, (nb + 1) * 512)
            xb, txb = xring.next()
            for c in range(8):
                k.dma("pool", xb[:, c, :], xTv[:, c, ts_], writes=[txb])
            for i in range(32):
                if i < 28:
                    c0, m = i * 96, 96
                    dst = QT[i] if i < 16 else KT[i - 16]
                else:
                    c0, m = 2688 + (i - 28) * 64, 64
                    dst = VcT[i - 28]
                ps, tps = psring.next()
                mm_acc(k, ps[:m, :], tps, [(wf[:, kc, c0:c0 + m], xb[:, kc, :]) for kc in range(8)], [twf, txb])
                ob, tob = oring.next()
                evac(k, ev[nev % 2], ob[:m, :], ps[:m, :], [tps], [tob]); nev += 1
                k.dma("sp", dst[:, ts_], ob[:m, :], reads=[tob])
            for ts in range(4):
                tsl = slice(ts * 128, (ts + 1) * 128)
                ps, tps = psring.next()
                mm_acc(k, ps[:], tps, [(xb[:, kc, tsl], wt[:, kc, 0:512]) for kc in range(8)], [twt, txb])
                ob, tob = oring.next()
                evac(k, ev[nev % 2], ob[:], ps[:], [tps], [tob]); nev += 1
                k.dma("sp", Vtm[nb * 512 + ts * 128:nb * 512 + (ts + 1) * 128, :], ob[:], reads=[tob])
                ps, tps = psring.next()
                mm_acc(k, ps[:, 0:48], tps, [(xb[:, kc, tsl], wt[:, kc, 512:560]) for kc in range(8)], [twt, txb])
                gb, tgb = gring.next()
                k.op("act", lambda e: e.activation(out=gb[:], in_=ps[:, 0:48], func=AF.Sigmoid), reads=[tps], writes=[tgb])
                k.dma("sp", gate[nb * 512 + ts * 128:nb * 512 + (ts + 1) * 128, :], gb[:], reads=[tgb])
        k.finish()
    return nc


def prep_D(I):
    w = I["nsa_w_in"][0]
    q, kc, vc, ks, vs, kw, vw, gt = np.split(w, [1536, 1920, 2176, 2560, 2816, 3200, 3456], axis=1)
    w_fm = np.ascontiguousarray(np.concatenate([q, kc, ks, kw, vc], 1))
    w_tm = np.ascontiguousarray(np.concatenate([vs, vw, gt], 1))
    return dict(w_fm=w_fm, w_tm=w_tm)


NSA_SCALE = 96 ** -0.5
NEG = -30000.0
FORCE = 1e6
GC = 1.5957691216057308


def pipeline(tiles, stages):
    n = len(tiles)
    ns = len(stages)
    for step in range(n + ns - 1):
        for s, fn in enumerate(stages):
            i = step - s
            if 0 <= i < n:
                fn(tiles[i])


def build_E(S=8192, NB=2):
    nc = bass.Bass("TRN2", target_bir_lowering=False)
    dt = lambda n, s, d, kind: nc.dram_tensor(n, s, d, kind=kind).ap()
    NQT = S // 128
    QTt = dt("QTt", [NB, NQT, 96, 512], BF16, "ExternalInput")
    KT = dt("KT", [NB, 3, 96, S], BF16, "ExternalInput")
    VcT = dt("VcT", [NB, 64, S], BF16, "ExternalInput")
    Vs = dt("Vs", [NB, S, 64], BF16, "ExternalInput")
    Vw = dt("Vw", [NB, S, 64], BF16, "ExternalInput")
    gate = dt("gate", [NB, S, 12], F32, "ExternalInput")
    w1k = dt("w1k", [96, 32, 96], F32, "ExternalInput")
    w2k = dt("w2k", [96, 96], F32, "ExternalInput")
    pekT = dt("pekT", [96, 32, 2], F32, "ExternalInput")
    w1v = dt("w1v", [64, 32, 64], F32, "ExternalInput")
    w2v = dt("w2v", [64, 64], F32, "ExternalInput")
    pevT = dt("pevT", [64, 32, 2], F32, "ExternalInput")
    ovl = dt("ovl", [128, 4, 128], BF16, "ExternalInput")
    Emat = dt("Emat", [128, NQT, 128], BF16, "ExternalInput")
    ABm = dt("ABm", [128, 2, 254], F32, "ExternalInput")
    raw = dt("raw", [19, 128, 512], F32, "ExternalInput")
    nmask = dt("nmask", [20, 128, 512], F32, "ExternalInput")
    rb31 = dt("rb31", [128, 512], F32, "ExternalInput")
    ident = dt("ident", [128, 128], F32, "ExternalInput")
    O = dt("O", [NB, S, 256], BF16, "ExternalOutput")
    with ExitStack() as es:
        k = KB(nc, es)
        ones = k.sb([128, 128], BF16); tones = Tok()
        k.op("dve", lambda e: e.memset(ones[:], 1.0), writes=[tones])
        w1k_t = k.sb([96, 32, 96], BF16); tw1k = Tok()
        k.dma("pool", w1k_t[:], w1k, writes=[tw1k])
        w2k_t = k.sb([96, 96], BF16); tw2k = Tok()
        k.dma("pool", w2k_t[:], w2k, writes=[tw2k])
        pek_t = k.sb([96, 32, 2], BF16); tpek = Tok()
        k.dma("pool", pek_t[:], pekT, writes=[tpek])
        w1v_t = k.sb([64, 32, 64], BF16); tw1v = Tok()
        k.dma("pool", w1v_t[:], w1v, writes=[tw1v])
        w2v_t = k.sb([64, 64], BF16); tw2v = Tok()
        k.dma("pool", w2v_t[:], w2v, writes=[tw2v])
        pev_t = k.sb([64, 32, 2], BF16); tpev = Tok()
        k.dma("pool", pev_t[:], pevT, writes=[tpev])
        E_t = k.sb([128, NQT, 128], BF16); tE = Tok()
        k.dma("sp", E_t[:], Emat, writes=[tE])
        AB_t = k.sb([128, 2, 254], F32); tAB = Tok()
        k.dma("sp", AB_t[:], ABm, writes=[tAB])
        id_t = k.sb([128, 128], F32); tid = Tok()
        k.dma("sp", id_t[:], ident, writes=[tid])
        rhsC = k.sb([128, 4, 193], BF16); trhsC = Tok()
        k.op("pool", lambda e: e.memset(rhsC[:, :, 64:65], 1.0), writes=[trhsC])
        k.dma("sp", rhsC[:, :, 65:193], ovl, writes=[trhsC])
        tabs = k.sb([128, 20, 512], F32); ttabs = Tok()
        rb31_t = k.sb([128, 512], F32); trb31 = Tok()
        k.dma("sp", rb31_t[:], rb31, writes=[trb31])
        stg = sb_ring(k, 2, [128, 512], F32)
        stg2 = sb_ring(k, 2, [128, 512], F32)
        for t in range(19):
            r_, tr_ = stg.next()
            m_, tm_ = stg2.next()
            k.dma("sp", r_[:], raw[t], writes=[tr_])
            k.dma("sp", m_[:], nmask[t], writes=[tm_])
            k.op("dve", lambda e: e.tensor_tensor(out=r_[:], in0=r_[:], in1=rb31_t[:], op=ALU.subtract),
                 reads=[tr_, trb31], writes=[tr_])
            k.op("dve", lambda e: e.scalar_tensor_tensor(out=tabs[:, t, :], in0=r_[:], scalar=1.0 / NSA_SCALE, in1=m_[:],
                                                         op0=ALU.mult, op1=ALU.add), reads=[tr_, tm_], writes=[ttabs])
        k.dma("sp", tabs[:, 19, :], nmask[19], writes=[ttabs])
        kc = k.sb([96, S], BF16); tkc = Tok()
        ks = k.sb([96, S], BF16); tks = Tok()
        kw = k.sb([96, S], BF16); tkw = Tok()
        vc = k.sb([64, S], BF16); tvc = Tok()
        vs1 = k.sb([128, NQT, 65], BF16); tvs1 = Tok()
        vw1 = k.sb([128, NQT, 65], BF16); tvw1 = Tok()
        k.op("pool", lambda e: e.memset(vs1[:, :, 64:65], 1.0), writes=[tvs1])
        k.op("pool", lambda e: e.memset(vw1[:, :, 64:65], 1.0), writes=[tvw1])
        gate_t = k.sb([128, NQT, 12], F32); tgate = Tok()
        kcmp = k.sb([96, 512], BF16); tkcmp = Tok()
        gx = k.sb([96, 512], F32); tgx = Tok()
        gu = k.sb([96, 512], F32); tgu = Tok()
        gl = k.sb([96, 512], BF16); tgl = Tok()
        k.op("pool", lambda e: e.memset(gl[:], 0.0), writes=[tgl])
        cb = k.sb([96, 2], F32); tcb = Tok()
        sqr = sb_ring(k, 3, [128, 512], BF16)
        mx = k.sb([128, 64], F32); tmx = Tok()
        qm2 = k.sb([128, 1], F32); tqm2 = Tok()
        km2 = k.sb([128, 1], F32); tkm2 = Tok()
        negM = [k.sb([128, 1], F32) for _ in range(3)]
        tnegM = [Tok() for _ in range(3)]
        sring = ps_ring(k, 3)
        accs = [k.ps([128, 512]) for _ in range(4)]
        taccs = [Tok() for _ in range(4)]
        misc = k.ps([128, 512])
        pse_ring = Ring([misc[:, 0:128], misc[:, 128:256]])
        psT = misc[:, 256:384]; tpsT = Tok()
        qring = sb_ring(k, 3, [96, 512], BF16)
        pring = sb_ring(k, 3, [128, 512], BF16)
        sbring = sb_ring(k, 3, [128, 512], F32)
        mkring = sb_ring(k, 3, [128, 128], F32)
        l4 = k.sb([128, 4], F32); tl4 = Tok()
        f4 = k.sb([128, 4], F32); tf4 = Tok()
        imp = k.sb([128, 128], F32); timp = Tok()
        imp2 = k.sb([128, 128], F32); timp2 = Tok()
        m8 = k.sb([128, 16], F32); tm8 = Tok()
        negsel = k.sb([128, 128], F32); tnegsel = Tok()
        negselT = k.sb([128, 128], BF16); tnegselT = Tok()
        oacc = k.sb([128, 4, 64], F32); toacc = Tok()
        obring = sb_ring(k, 2, [128, 256], BF16)

        def maxnorm(a, ta, np_, nblk, res, tres, col0=0):
            for i in range(nblk):
                sl = slice(i * 512, (i + 1) * 512)
                s1, ts1 = sqr.next()
                k.op("dve", lambda e: e.tensor_tensor(out=s1[:np_, :], in0=a[:np_, sl], in1=a[:np_, sl], op=ALU.mult),
                     reads=[ta], writes=[ts1])
                ps, tps = sring.next()
                mm_acc(k, ps[:], tps, [(ones[:np_, :], s1[:np_, :])], [tones, ts1])
                k.op("dve", lambda e: e.reduce_max(out=mx[:, col0 + i:col0 + i + 1], in_=ps[:], axis=AX.X),
                     reads=[tps], writes=[tmx])
            if res is not None:
                k.op("dve", lambda e: e.reduce_max(out=res[:], in_=mx[:, 0:col0 + nblk], axis=AX.X), reads=[tmx], writes=[tres])

        def set_negM(i):
            k.op("dve", lambda e: e.tensor_tensor(out=negM[i][:], in0=qm2[:], in1=km2[:], op=ALU.mult),
                 reads=[tqm2, tkm2, tnegM[i]], writes=[tnegM[i]])
            k.op("act", lambda e: e.activation(out=negM[i][:], in_=negM[i][:], func=AF.Sqrt), reads=[tnegM[i]], writes=[tnegM[i]])
            k.op("dve", lambda e: e.tensor_scalar(out=negM[i][:], in0=negM[i][:], scalar1=-NSA_SCALE * 1.05, scalar2=-4.0,
                                                  op0=ALU.mult, op1=ALU.add), reads=[tnegM[i]], writes=[tnegM[i]])

        def compress(src, tsrc, nd, w1_t, tw1, pe_t, tpe):
            sv = src[:nd, :].rearrange("d (n s) -> d n s", s=16)
            ps, tps = sring.next()
            pairs = []
            for l in range(32):
                rhs = sv[:, 0:511, l] if l < 16 else sv[:, 1:512, l - 16]
                pairs.append((w1_t[:nd, l, :], rhs))
            mm_acc(k, ps[:nd, 0:511], tps, pairs, [tw1, tsrc])
            ps2, tps2 = sring.next()
            mm_acc(k, ps2[:nd, 0:2], tps2, [(w1_t[:nd, l, :], pe_t[:nd, l, :]) for l in range(32)], [tw1, tpe])
            k.op("act", lambda e: e.activation(out=cb[:nd, :], in_=ps2[:nd, 0:2], func=AF.Copy), reads=[tps2], writes=[tcb])
            k.op("act", lambda e: e.activation(out=gx[:nd, 0:511], in_=ps[:nd, 0:511], func=AF.Identity, bias=cb[:nd, 0:1]),
                 reads=[tps, tcb], writes=[tgx])
            k.op("dve", lambda e: e.tensor_tensor(out=gu[:nd, 0:511], in0=gx[:nd, 0:511], in1=gx[:nd, 0:511], op=ALU.mult),
                 reads=[tgx], writes=[tgu])
            k.op("dve", lambda e: e.tensor_scalar(out=gu[:nd, 0:511], in0=gu[:nd, 0:511], scalar1=0.044715, scalar2=1.0,
                                                  op0=ALU.mult, op1=ALU.add), reads=[tgu], writes=[tgu])
            k.op("dve", lambda e: e.tensor_tensor(out=gu[:nd, 0:511], in0=gu[:nd, 0:511], in1=gx[:nd, 0:511], op=ALU.mult),
                 reads=[tgu, tgx], writes=[tgu])
            k.op("act", lambda e: e.activation(out=gu[:nd, 0:511], in_=gu[:nd, 0:511], func=AF.Sigmoid, scale=GC),
                 reads=[tgu], writes=[tgu])
            k.op("dve", lambda e: e.tensor_tensor(out=gl[:nd, 0:511], in0=gu[:nd, 0:511], in1=gx[:nd, 0:511], op=ALU.mult),
                 reads=[tgu, tgx], writes=[tgl])

        for bi in range(NB):
            k.dma("sp", kc[:], KT[bi, 0], writes=[tkc])
            k.dma("sp", ks[:], KT[bi, 1], writes=[tks])
            k.dma("sp", kw[:], KT[bi, 2], writes=[tkw])
            k.dma("sp", vc[:], VcT[bi], writes=[tvc])
            k.dma("sp", vs1[:, :, 0:64], Vs[bi].rearrange("(t p) d -> p t d", p=128), writes=[tvs1])
            k.dma("sp", vw1[:, :, 0:64], Vw[bi].rearrange("(t p) d -> p t d", p=128), writes=[tvw1])
            k.dma("sp", gate_t[:], gate[bi].rearrange("(t p) c -> p t c", p=128), writes=[tgate])
            compress(kc, tkc, 96, w1k_t, tw1k, pek_t, tpek)
            ps, tps = sring.next()
            mm_acc(k, ps[:96, :], tps, [(w2k_t[:, :], gl[:96, :])], [tw2k, tgl])
            k.op("act", lambda e: e.activation(out=kcmp[:], in_=ps[:96, :], func=AF.Copy), reads=[tps], writes=[tkcmp])
            compress(vc, tvc, 64, w1v_t, tw1v, pev_t, tpev)
            for nt in range(4):
                ps, tps = sring.next()
                mm_acc(k, ps[:, 0:64], tps, [(gl[:64, nt * 128:(nt + 1) * 128], w2v_t[:, :])], [tw2v, tgl])
                k.op("act", lambda e: e.activation(out=rhsC[:, nt, 0:64], in_=ps[:, 0:64], func=AF.Copy),
                     reads=[tps], writes=[trhsC])
            for qt in range(NQT):
                qm, tqm = qring.next()
                k.dma("sp", qm[:], QTt[bi, qt], writes=[tqm])
                maxnorm(qm, tqm, 96, 1, qm2 if qt == NQT - 1 else None, tqm2, col0=qt)
            for i, (kk, tkk, nblk) in enumerate([(kcmp, tkcmp, 1), (ks, tks, S // 512), (kw, tkw, S // 512)]):
                maxnorm(kk, tkk, 96, nblk, km2, tkm2)
                set_negM(i)

            def branch(qt, qm, tqm, tiles, kT, tkT, tabsel, usemask, rhs_of, trhs, ncol, nm, tnm, gcol, first, last_out):
                ctx = {}

                def stA(kt):
                    sps, tsp = sring.next()
                    if usemask:
                        pe_, tpe_ = pse_ring.next()
                        k.op("pe", lambda e: e.matmul(pe_, lhsT=E_t[:, kt, :], rhs=negselT[:], start=True, stop=True),
                             reads=[tE, tnegselT], writes=[tpe_])
                    mm_acc(k, sps[:], tsp, [(kT[:, kt * 128:(kt + 1) * 128], qm[:])], [tkT, tqm])
                    mk = tmk = None
                    if usemask:
                        mk, tmk = mkring.next()
                        k.op("act", lambda e: e.activation(out=mk[:], in_=pe_, func=AF.Copy), reads=[tpe_], writes=[tmk])
                    ctx[kt] = [sps, tsp, mk, tmk]

                def stB(kt):
                    sps, tsp, mk, tmk = ctx[kt]
                    tb = tabsel(kt)
                    src, tsrc = sps, tsp
                    if usemask:
                        sb, tsb = sbring.next()
                        k.op("dve", lambda e: e.tensor_tensor(out=sb[:].rearrange("p (h q) -> p h q", h=4),
                                                              in0=sps[:].rearrange("p (h q) -> p h q", h=4),
                                                              in1=mk[:].unsqueeze(1).to_broadcast([128, 4, 128]), op=ALU.add),
                             reads=[tsp, tmk], writes=[tsb])
                        if tb is not None:
                            k.op("pool", lambda e: e.tensor_tensor(out=sb[:], in0=sb[:], in1=tabs[:, tb, :], op=ALU.add),
                                 reads=[tsb, ttabs], writes=[tsb])
                        src, tsrc = sb, tsb
                    elif tb is not None:
                        sb, tsb = sbring.next()
                        k.op("dve", lambda e: e.tensor_tensor(out=sb[:], in0=sps[:], in1=tabs[:, tb, :], op=ALU.add),
                             reads=[tsp, ttabs], writes=[tsb])
                        src, tsrc = sb, tsb
                    pt, tpt = pring.next()
                    k.op("act", lambda e: e.activation(out=pt[:], in_=src[:], func=AF.Exp, scale=NSA_SCALE, bias=nm[:]),
                         reads=[tsrc, tnm], writes=[tpt])
                    ctx[kt] = [pt, tpt]

                def stC(kt):
                    pt, tpt = ctx.pop(kt)
                    for h in range(4):
                        k.op("pe", lambda e, h=h: e.matmul(accs[h][:, 0:ncol], lhsT=pt[:, h * 128:(h + 1) * 128],
                                                           rhs=rhs_of(kt), start=(kt == tiles[0]), stop=(kt == tiles[-1])),
                             reads=[tpt, trhs], writes=[taccs[h]])

                pipeline(tiles, [stA, stB, stC])
                for h in range(4):
                    k.op("dve", lambda e, h=h: e.tensor_scalar(out=l4[:, h:h + 1], in0=accs[h][:, 64:65], scalar1=1e-30,
                                                               scalar2=None, op0=ALU.max), reads=[taccs[h]], writes=[tl4])
                k.op("dve", lambda e: e.reciprocal(out=l4[:], in_=l4[:]), reads=[tl4], writes=[tl4])
                gv = gate_t[:, qt, :].rearrange("p (h c) -> p h c", c=3)[:, :, gcol]
                k.op("dve", lambda e: e.tensor_tensor(out=f4[:], in0=l4[:], in1=gv, op=ALU.mult),
                     reads=[tl4, tgate], writes=[tf4])
                if first:
                    k.op("dve", lambda e: e.tensor_scalar(out=imp[:], in0=accs[0][:, 65:193], scalar1=l4[:, 0:1], scalar2=None,
                                                          op0=ALU.mult), reads=[taccs[0], tl4], writes=[timp])
                    for h in range(1, 4):
                        k.op("dve", lambda e, h=h: e.scalar_tensor_tensor(out=imp[:], in0=accs[h][:, 65:193], scalar=l4[:, h:h + 1],
                                                                          in1=imp[:], op0=ALU.mult, op1=ALU.add),
                             reads=[taccs[h], tl4, timp], writes=[timp])
                for h in range(4):
                    if first:
                        k.op("dve", lambda e, h=h: e.tensor_scalar(out=oacc[:, h, :], in0=accs[h][:, 0:64], scalar1=f4[:, h:h + 1],
                                                                   scalar2=None, op0=ALU.mult),
                             reads=[taccs[h], tf4], writes=[toacc])
                    elif not last_out:
                        k.op("dve", lambda e, h=h: e.scalar_tensor_tensor(out=oacc[:, h, :], in0=accs[h][:, 0:64],
                                                                          scalar=f4[:, h:h + 1], in1=oacc[:, h, :],
                                                                          op0=ALU.mult, op1=ALU.add),
                             reads=[taccs[h], tf4, toacc], writes=[toacc])
                    else:
                        ob, tob = last_out
                        k.op("dve", lambda e, h=h: e.scalar_tensor_tensor(out=ob[:, h * 64:(h + 1) * 64], in0=accs[h][:, 0:64],
                                                                          scalar=f4[:, h:h + 1], in1=oacc[:, h, :],
                                                                          op0=ALU.mult, op1=ALU.add),
                             reads=[taccs[h], tf4, toacc], writes=[tob])

            for qt in range(NQT):
                qm, tqm = qring.next()
                k.dma("sp", qm[:], QTt[bi, qt], writes=[tqm])
                ntmax = (8 * qt + 6) // 128
                branch(qt, qm, tqm, list(range(ntmax + 1)), kcmp, tkcmp,
                       lambda nt: (qt - 16 * nt) if (qt - 16 * nt) <= 16 else None, False,
                       lambda nt: rhsC[:, nt, :], trhsC, 193, negM[0], tnegM[0], 0, True, None)
                a0 = 126 - 2 * qt
                k.op("dve", lambda e: e.tensor_tensor(out=imp2[:], in0=imp[:], in1=AB_t[:, 0, a0:a0 + 128], op=ALU.mult),
                     reads=[timp, tAB], writes=[timp2])
                k.op("dve", lambda e: e.tensor_tensor(out=imp2[:], in0=imp2[:], in1=AB_t[:, 1, a0:a0 + 128], op=ALU.add),
                     reads=[timp2, tAB], writes=[timp2])
                k.op("dve", lambda e: e.memset(imp2[:, 0:1], FORCE), reads=[timp2], writes=[timp2])
                k.op("dve", lambda e: e.max(out=m8[:, 0:8], in_=imp2[:]), reads=[timp2], writes=[tm8])
                k.op("dve", lambda e: e.match_replace(out=imp[:], in_to_replace=m8[:, 0:8], in_values=imp2[:], imm_value=-3e38),
                     reads=[timp2, tm8], writes=[timp])
                k.op("dve", lambda e: e.max(out=m8[:, 8:16], in_=imp[:]), reads=[timp], writes=[tm8])
                k.op("dve", lambda e: e.tensor_scalar(out=imp2[:], in0=imp2[:], scalar1=m8[:, 15:16], scalar2=None, op0=ALU.is_ge),
                     reads=[timp2, tm8], writes=[timp2])
                k.op("dve", lambda e: e.tensor_scalar(out=negsel[:], in0=imp2[:], scalar1=-NEG, scalar2=NEG, op0=ALU.mult,
                                                      op1=ALU.add), reads=[timp2], writes=[tnegsel])
                k.op("pe", lambda e: e.transpose(psT, negsel[:], id_t[:]), reads=[tnegsel, tid], writes=[tpsT])
                k.op("act", lambda e: e.activation(out=negselT[:], in_=psT, func=AF.Copy), reads=[tpsT], writes=[tnegselT])
                branch(qt, qm, tqm, list(range(qt + 1)), ks, tks,
                       lambda kt: 17 if kt == qt else (18 if kt == qt - 1 else None), True,
                       lambda kt: vs1[:, kt, :], tvs1, 65, negM[1], tnegM[1], 1, False, None)
                ob, tob = obring.next()
                branch(qt, qm, tqm, list(range(max(0, qt - 4), qt + 1)), kw, tkw,
                       lambda kt: 17 if kt == qt else (18 if kt == qt - 1 else (19 if kt == qt - 4 else None)), False,
                       lambda kt: vw1[:, kt, :], tvw1, 65, negM[2], tnegM[2], 2, False, (ob, tob))
                k.dma("sp", O[bi, qt * 128:(qt + 1) * 128, :], ob[:], reads=[tob])
        k.finish()
    return nc


def rel_bucket_np(dist):
    n = np.maximum(dist, 0)
    nf = np.maximum(n, 1).astype(np.float32)
    large = 16 + (np.log(nf / np.float32(16)) / np.float32(np.log(128 / 16)) * np.float32(16)).astype(np.int32)
    large = np.minimum(large, 31)
    return np.where(n < 16, n, large)


def consts_E(S=8192):
    NQT = S // 128
    n = np.arange(512)
    j = np.arange(128)
    ov = ((16 * n[:, None] <= 64 * j[None, :] + 63) & (16 * n[:, None] + 31 >= 64 * j[None, :])).astype(np.float32)
    ov[511] = 0
    ovl = np.ascontiguousarray(ov.reshape(4, 128, 128).transpose(1, 0, 2)).astype(NPBF)
    E = np.zeros((128, NQT, 128), np.float32)
    kk = np.arange(128)
    for kt in range(NQT):
        E[2 * kt + kk // 64, kt, kk] = 1
    q = np.arange(128)
    c = (q >= 64).astype(np.int64)[:, None]
    r = (np.arange(254) - 126)[None, :]
    A = np.where(r > c, 0.0, np.where((r == c) | (r == c - 1), 0.0, 1.0))
    Bt = np.where(r > c, -FORCE, np.where((r == c) | (r == c - 1), FORCE, 0.0))
    ABm = np.ascontiguousarray(np.stack([A, Bt], 1)).astype(np.float32)
    p = np.arange(128)[:, None]
    qq = np.arange(128)[None, :]
    dists = [qq - 16 * p + 128 * off - 31 for off in range(17)] + [qq - p, qq - p + 128]
    dist = np.stack(dists, 0)
    bucket = rel_bucket_np(dist)
    nm = np.where(dist < 0, NEG, 0.0).astype(np.float32)
    edge = np.where(p <= qq, NEG, 0.0).astype(np.float32)
    nm = np.concatenate([nm, edge[None]], 0)
    nmask = np.ascontiguousarray(np.broadcast_to(nm[:, :, None, :], (20, 128, 4, 128)).reshape(20, 128, 512))
    return dict(ovl=ovl, Emat=E.astype(NPBF), ABm=ABm, nmask=nmask, ident=np.eye(128, dtype=np.float32)), bucket


def prep_E(I, g, bucket):
    rb = I["rel_bias"]
    heads = 4 * g + np.arange(4)
    raw = rb[bucket][:, :, :, heads]
    raw = np.ascontiguousarray(raw.transpose(0, 1, 3, 2).reshape(19, 128, 512))
    rb31 = np.ascontiguousarray(np.broadcast_to(rb[31, heads][None, :, None], (128, 4, 128)).reshape(128, 512))
    w1k = np.ascontiguousarray(I["nsa_cmp_w1_k"][0].transpose(1, 0, 2))
    w1v = np.ascontiguousarray(I["nsa_cmp_w1_v"][0].transpose(1, 0, 2))
    pek = np.ascontiguousarray(np.repeat(I["nsa_cmp_pe_k"][0].T[:, :, None], 2, 2))
    pev = np.ascontiguousarray(np.repeat(I["nsa_cmp_pe_v"][0].T[:, :, None], 2, 2))
    return dict(raw=raw, rb31=rb31, w1k=w1k, w2k=I["nsa_cmp_w2_k"][0], pekT=pek, w1v=w1v, w2v=I["nsa_cmp_w2_v"][0], pevT=pev)


def maps_E(I, D, S=8192):
    cst, bucket = consts_E(S)
    maps = []
    for c in range(8):
        g, bs = c % 4, [2 * (c // 4), 2 * (c // 4) + 1]
        m = dict(cst)
        m.update(prep_E(I, g, bucket))
        QTt, KT, VcT, Vs, Vw, gt = [], [], [], [], [], []
        for b in bs:
            cat = lambda n, ax: np.concatenate([D[f"{n}_{2 * b}"], D[f"{n}_{2 * b + 1}"]], axis=ax)
            QT = cat("QT", 2)[4 * g:4 * g + 4]
            QTt.append(QT.reshape(4, 96, S // 128, 128).transpose(2, 1, 0, 3).reshape(S // 128, 96, 512))
            K = cat("KT", 2)
            KT.append(np.stack([K[g], K[4 + g], K[8 + g]], 0))
            VcT.append(cat("VcT", 2)[g])
            V = cat("Vtm", 0)
            Vs.append(V[:, g * 64:(g + 1) * 64]); Vw.append(V[:, 256 + g * 64:256 + (g + 1) * 64])
            gt.append(cat("gate", 0)[:, g * 12:(g + 1) * 12])
        m.update(QTt=np.ascontiguousarray(np.stack(QTt)), KT=np.ascontiguousarray(np.stack(KT)),
                 VcT=np.ascontiguousarray(np.stack(VcT)), Vs=np.ascontiguousarray(np.stack(Vs)),
                 Vw=np.ascontiguousarray(np.stack(Vw)), gate=np.ascontiguousarray(np.stack(gt)))
        maps.append(m)
    return maps


def build_F(NT=4096, HALF=2048, NB=256):
    nc = bass.Bass("TRN2", target_bir_lowering=False)
    dt = lambda n, s, d, kind: nc.dram_tensor(n, s, d, kind=kind).ap()
    OT = dt("OT", [1024, NT], BF16, "ExternalInput")
    xT = dt("xT", [1024, NT], F32, "ExternalInput")
    w_out = dt("w_out", [1024, 1024], F32, "ExternalInput")
    lnp = dt("lnp", [128, 4, 8], F32, "ExternalInput")
    w_r = dt("w_r", [1024, 8], F32, "ExternalInput")
    w_g = dt("w_g", [8, 1024, 3584], F32, "ExternalInput")
    w_u = dt("w_u", [8, 1024, 3584], F32, "ExternalInput")
    w_d = dt("w_d", [8, 3584, 1024], F32, "ExternalInput")
    sel = dt("sel", [8, 8, 128], BF16, "ExternalInput")
    ident = dt("ident", [128, 128], F32, "ExternalInput")
    x3T = dt("x3T", [1024, NT], F32, "ExternalOutput")
    outT = dt("outT", [1024, NT], F32, "ExternalOutput")
    NFG = 7
    with ExitStack() as es:
        k = KB(nc, es)
        wo, two = load_w(k, w_out, 8, 1024)
        ln_t = k.sb([128, 4, 8], F32); tln = Tok()
        k.dma("sp", ln_t[:], lnp, writes=[tln])
        wr = k.sb([128, 8, 8], F32); twr = Tok()
        k.dma("sp", wr[:], w_r.rearrange("(c p) e -> p c e", p=128), writes=[twr])
        sel_t = k.sb([8, 8, 128], BF16); tsel = Tok()
        k.dma("sp", sel_t[:], sel, writes=[tsel])
        id_t = k.sb([128, 128], F32); tid = Tok()
        k.dma("sp", id_t[:], ident, writes=[tid])
        ones = k.sb([128, 128], BF16); tones = Tok()
        k.op("dve", lambda e: e.memset(ones[:], 1.0), writes=[tones])
        psring = ps_ring(k, 7)
        sqring = sb_ring(k, 4, [128, NB], BF16)
        ob = k.sb([128, 8, NB], BF16); tob = Tok()
        y = k.sb([128, 8, NB], F32); ty = Tok()
        mean = k.sb([128, NB], F32); tmean = Tok()
        rstd = k.sb([128, NB], F32); trstd = Tok()
        tmp = k.sb([128, NB], F32); ttmp = Tok()
        x3b = k.sb([128, 8, HALF], BF16); tx3b = Tok()
        yacc = k.sb([128, 8, HALF], F32); tyacc = Tok()
        wtsT = k.sb([8, HALF], BF16); twtsT = Tok()
        lg = k.sb([128, 8], F32); tlg = Tok()
        m8 = k.sb([128, 8], F32); tm8 = Tok()
        pp = k.sb([128, 4], F32); tpp = Tok()
        w1 = k.sb([128, 8], F32); tw1 = Tok()
        w2 = k.sb([128, 8], F32); tw2 = Tok()
        wgr = sb_ring(k, 2, [128, 8, 512], BF16)
        wur = sb_ring(k, 2, [128, 8, 512], BF16)
        wdr = sb_ring(k, 2, [128, 4, 1024], BF16)
        hwr = sb_ring(k, 2, [128, 4, 512], BF16)
        sgr = sb_ring(k, 3, [128, 512], BF16)
        hur = sb_ring(k, 3, [128, 512], BF16)
        wbr = sb_ring(k, 2, [128, 512], BF16)
        tx3d = Tok()
        OTv = OT.rearrange("(c p) t -> p c t", p=128)
        xTv = xT.rearrange("(c p) t -> p c t", p=128)
        x3v = x3T.rearrange("(c p) t -> p c t", p=128)
        outv = outT.rearrange("(c p) t -> p c t", p=128)
        for hf in range(NT // HALF):
            for sbk in range(HALF // NB):
                g0 = hf * HALF + sbk * NB
                ts_ = slice(g0, g0 + NB)
                ls_ = slice(sbk * NB, (sbk + 1) * NB)
                k.dma("sp", ob[:], OTv[:, :, ts_], writes=[tob])
                k.dma("sp", y[:], xTv[:, :, ts_], writes=[ty])
                for m in range(8):
                    ps, tps = psring.next()
                    mm_acc(k, ps[:, :NB], tps, [(wo[:, kc, m * 128:(m + 1) * 128], ob[:, kc, :]) for kc in range(8)], [two, tob])
                    k.op("dve", lambda e: e.scalar_tensor_tensor(out=y[:, m, :], in0=y[:, m, :], scalar=ALPHA, in1=ps[:, :NB],
                                                                 op0=ALU.mult, op1=ALU.add), reads=[ty, tps], writes=[ty])
                ln_fm(k, psring, sqring, ones, tones, y, ty, NB, ln_t[:, 0, :], ln_t[:, 1, :], tln, mean, tmean, rstd, trstd,
                      tmp, ttmp, [])
                for m in range(8):
                    k.op("pool", lambda e: e.tensor_copy(out=x3b[:, m, ls_], in_=y[:, m, :]), reads=[ty], writes=[tx3b])
                k.dma("sp", x3v[:, :, ts_], y[:], reads=[ty], writes=[tx3d])
                for tt in range(NB // 128):
                    tsl = slice(tt * 128, (tt + 1) * 128)
                    ps, tps = psring.next()
                    mm_acc(k, ps[:, 0:8], tps, [(y[:, kc, tsl], wr[:, kc, :]) for kc in range(8)], [ty, twr])
                    k.op("act", lambda e: e.activation(out=lg[:], in_=ps[:, 0:8], func=AF.Copy), reads=[tps], writes=[tlg])
                    k.op("dve", lambda e: e.max(out=m8[:], in_=lg[:]), reads=[tlg], writes=[tm8])
                    k.op("dve", lambda e: e.tensor_tensor(out=pp[:, 0:1], in0=m8[:, 0:1], in1=m8[:, 1:2], op=ALU.subtract),
                         reads=[tm8], writes=[tpp])
                    k.op("act", lambda e: e.activation(out=pp[:, 1:2], in_=pp[:, 0:1], func=AF.Sigmoid), reads=[tpp], writes=[tpp])
                    k.op("act", lambda e: e.activation(out=pp[:, 2:3], in_=pp[:, 0:1], func=AF.Sigmoid, scale=-1.0),
                         reads=[tpp], writes=[tpp])
                    k.op("dve", lambda e: e.tensor_scalar(out=w1[:], in0=lg[:], scalar1=m8[:, 0:1], scalar2=pp[:, 1:2],
                                                          op0=ALU.is_equal, op1=ALU.mult), reads=[tlg, tm8, tpp], writes=[tw1])
                    k.op("dve", lambda e: e.tensor_scalar(out=w2[:], in0=lg[:], scalar1=m8[:, 1:2], scalar2=pp[:, 2:3],
                                                          op0=ALU.is_equal, op1=ALU.mult), reads=[tlg, tm8, tpp], writes=[tw2])
                    k.op("dve", lambda e: e.tensor_tensor(out=w1[:], in0=w1[:], in1=w2[:], op=ALU.add), reads=[tw1, tw2], writes=[tw1])
                    ps2, tps2 = psring.next()
                    k.op("pe", lambda e: e.transpose(ps2[0:8, 0:128], w1[:], id_t[:]), reads=[tw1, tid], writes=[tps2])
                    c0 = sbk * NB + tt * 128
                    k.op("act", lambda e: e.activation(out=wtsT[:, c0:c0 + 128], in_=ps2[0:8, 0:128], func=AF.Copy),
                         reads=[tps2], writes=[twtsT])
            for m in range(8):
                k.op("pool", lambda e: e.memset(yacc[:, m, :], 0.0), writes=[tyacc])
            for ex in range(8):
                for fg in range(NFG):
                    fs = slice(fg * 512, (fg + 1) * 512)
                    wg_t, twg = wgr.next()
                    wu_t, twu = wur.next()
                    wd_t, twd = wdr.next()
                    for kc in range(8):
                        k.dma("pool", wg_t[:, kc, :], w_g[ex, kc * 128:(kc + 1) * 128, fs], writes=[twg])
                        k.dma("pool", wu_t[:, kc, :], w_u[ex, kc * 128:(kc + 1) * 128, fs], writes=[twu])
                    for fc in range(4):
                        r0 = fg * 512 + fc * 128
                        k.dma("pool", wd_t[:, fc, :], w_d[ex, r0:r0 + 128, :], writes=[twd])
                    for tb in range(HALF // 512):
                        tsl = slice(tb * 512, (tb + 1) * 512)
                        psb, tpsb = psring.next()
                        mm_acc(k, psb[:], tpsb, [(sel_t[:, ex, :], wtsT[:, tsl])], [tsel, twtsT])
                        wb, twb = wbr.next()
                        k.op("act", lambda e: e.activation(out=wb[:], in_=psb[:], func=AF.Copy), reads=[tpsb], writes=[twb])
                        hw, thw = hwr.next()
                        for fc in range(4):
                            psg, tpg = psring.next()
                            mm_acc(k, psg[:], tpg, [(wg_t[:, kc, fc * 128:(fc + 1) * 128], x3b[:, kc, tsl]) for kc in range(8)],
                                   [twg, tx3b])
                            psu, tpu = psring.next()
                            mm_acc(k, psu[:], tpu, [(wu_t[:, kc, fc * 128:(fc + 1) * 128], x3b[:, kc, tsl]) for kc in range(8)],
                                   [twu, tx3b])
                            sg, tsg = sgr.next()
                            k.op("act", lambda e: e.activation(out=sg[:], in_=psg[:], func=AF.Silu), reads=[tpg], writes=[tsg])
                            hu, thu = hur.next()
                            k.op("dve", lambda e: e.tensor_tensor(out=hu[:], in0=sg[:], in1=psu[:], op=ALU.mult),
                                 reads=[tsg, tpu], writes=[thu])
                            k.op("pool", lambda e: e.tensor_tensor(out=hw[:, fc, :], in0=hu[:], in1=wb[:], op=ALU.mult),
                                 reads=[thu, twb], writes=[thw])
                        for m in range(8):
                            ps, tps = psring.next()
                            mm_acc(k, ps[:], tps, [(wd_t[:, fc, m * 128:(m + 1) * 128], hw[:, fc, :]) for fc in range(4)],
                                   [twd, thw])
                            k.op("dve", lambda e: e.tensor_tensor(out=yacc[:, m, tsl], in0=yacc[:, m, tsl], in1=ps[:], op=ALU.add),
                                 reads=[tyacc, tps], writes=[tyacc])
            for sbk in range(HALF // NB):
                g0 = hf * HALF + sbk * NB
                ts_ = slice(g0, g0 + NB)
                ls_ = slice(sbk * NB, (sbk + 1) * NB)
                k.dma("sp", y[:], x3v[:, :, ts_], reads=[tx3d], writes=[ty])
                for m in range(8):
                    k.op("dve", lambda e: e.scalar_tensor_tensor(out=y[:, m, :], in0=y[:, m, :], scalar=ALPHA, in1=yacc[:, m, ls_],
                                                                 op0=ALU.mult, op1=ALU.add), reads=[ty, tyacc], writes=[ty])
                ln_fm(k, psring, sqring, ones, tones, y, ty, NB, ln_t[:, 2, :], ln_t[:, 3, :], tln, mean, tmean, rstd, trstd,
                      tmp, ttmp, [])
                k.dma("sp", outv[:, :, ts_], y[:], reads=[ty])
        k.finish()
    return nc


def prep_F(I):
    lnp = np.stack([I["ln_mix_g"][1], I["ln_mix_b"][1], I["ln_ffn_g"][1], I["ln_ffn_b"][1]], 0)
    lnp = np.ascontiguousarray(lnp.reshape(4, 8, 128).transpose(2, 0, 1))
    sel = np.zeros((8, 8, 128), np.float32)
    for e in range(8):
        sel[e, e, :] = 1
    return dict(w_out=I["nsa_w_out"][0], lnp=lnp, w_r=I["moe_w_router"][0], w_g=I["moe_w_gate"][0], w_u=I["moe_w_up"][0],
                w_d=I["moe_w_down"][0], sel=sel.astype(NPBF), ident=np.eye(128, dtype=np.float32))


def maps_F(I, Eo, x2T, S=8192):
    com = prep_F(I)
    maps = []
    for c in range(8):
        b, hf = c // 2, c % 2
        O = np.concatenate([Eo[f"O_{4 * (b // 2) + g}"][b % 2] for g in range(4)], 1)
        OT = np.ascontiguousarray(O[hf * 4096:(hf + 1) * 4096].T)
        maps.append(dict(OT=OT, xT=x2T[c], **com))
    return maps


_CACHE = {}


def _run(name, builder, maps):
    if name not in _CACHE:
        _CACHE[name] = builder()
    res = run_bass_kernel_spmd(_CACHE[name], maps, core_ids=list(range(8)))
    return res.results


def maps_A(I):
    com = prep_A(I)
    maps = []
    for c in range(8):
        b, hf = c // 2, c % 2
        xs = np.ascontiguousarray(I["x"][b, hf * 4096:(hf + 1) * 4096, :].T)
        cosT, sinS = rope_tables(np.arange(hf * 4096, (hf + 1) * 4096))
        maps.append(dict(xT=xs, cosT=cosT, sinS=sinS, **com))
    return maps


def maps_B(A, S=8192):
    maps = []
    for c in range(8):
        b, hh = c // 2, c % 2
        cat = lambda n: np.concatenate([A[2 * b][n], A[2 * b + 1][n]], axis=-1)
        QnT = cat("QnT").reshape(8, 128, S)[hh * 4:hh * 4 + 4]
        QrT = cat("QrT").reshape(8, 64, S)[hh * 4:hh * 4 + 4]
        KnT = cat("KnT").reshape(8, 128, S)[hh * 4:hh * 4 + 4]
        KrT = cat("KrT")
        Vv = np.concatenate([A[2 * b]["V"], A[2 * b + 1]["V"]], 0).reshape(S, 8, 128)[:, hh * 4:hh * 4 + 4].transpose(1, 0, 2)
        maps.append(dict(QnT=np.ascontiguousarray(QnT), QrT=np.ascontiguousarray(QrT), KnT=np.ascontiguousarray(KnT),
                         KrT=np.ascontiguousarray(KrT), V=np.ascontiguousarray(Vv), tri=tri_mask()))
    return maps


def maps_C(I, Bo, mapsA):
    com = prep_C(I)
    maps = []
    for c in range(8):
        b, hf = c // 2, c % 2
        O = np.concatenate([Bo[2 * b]["O"], Bo[2 * b + 1]["O"]], 1)
        OT = np.ascontiguousarray(O[hf * 4096:(hf + 1) * 4096].T)
        maps.append(dict(OT=OT, xT=mapsA[c]["xT"], **com))
    return maps


def kernel(**inputs):
    I = {k_: np.asarray(v) for k_, v in inputs.items()}
    mA = maps_A(I)
    A = _run("A", build_A, mA)
    Bo = _run("B", build_B, maps_B(A))
    Co = _run("C", build_C, maps_C(I, Bo, mA))
    comD = prep_D(I)
    Do = _run("D", build_D, [dict(xT=Co[c]["x2T"], **comD) for c in range(8)])
    Dd = {f"{n}_{c}": Do[c][n] for c in range(8) for n in ["QT", "KT", "VcT", "Vtm", "gate"]}
    Eo = _run("E", build_E, maps_E(I, Dd))
    Ed = {f"O_{c}": Eo[c]["O"] for c in range(8)}
    Fo = _run("F", build_F, maps_F(I, Ed, [Co[c]["x2T"] for c in range(8)]))
    out = np.empty((4, 8192, 1024), np.float32)
    for c in range(8):
        b, hf = c // 2, c % 2
        out[b, hf * 4096:(hf + 1) * 4096, :] = Fo[c]["outT"].T
    return out
```
